# Optimizing a Trainium2 kernel written in Bass

```python
import math
import jax, jax.numpy as jnp
from jax import lax
import numpy as np

D_MODEL = 1024
BATCH = 2
SEQ = 8192
DEPTH = 2

GRID_W = 64
HEAD_DIM = 64
D_MIX = D_MODEL
C_WIDTH = D_MIX // 4
A_WIDTH = ((D_MIX - C_WIDTH) // 2 // HEAD_DIM) * HEAD_DIM
B_WIDTH = D_MIX - C_WIDTH - A_WIDTH
A_HEADS = A_WIDTH // HEAD_DIM
B_HEADS = B_WIDTH // HEAD_DIM
NA_KH = 8
NA_KW = 16
POOL_WINDOWS = (2, 4, 8, 16)
N_POOL_GROUPS = 4
POOL_CH = C_WIDTH // N_POOL_GROUPS
R_W = 32
R_A = 32
R_G = 64
DECAY_SCALE = math.exp(-0.5)
GN_EPS = 64e-5
IN_WIDTH = 3 * A_WIDTH + 3 * B_WIDTH + R_W + R_A + R_G + C_WIDTH
N_GROUPS = 4
EXPERTS_PER_GROUP = 8
N_EXPERTS = N_GROUPS * EXPERTS_PER_GROUP
TOP_K = 2
D_EXPERT = 512
MOE_BLOCK = 128
ALPHA = (2 * DEPTH) ** 0.25
BETA = (8 * DEPTH) ** -0.25
LN_EPS = 1e-5
NEG_INF = -1e30

kernel_name = "hybrid_natten_rwkv7_pool_hmoe_encoder"


def layer_norm(x, eps=LN_EPS):
    xf = x.astype(jnp.float32)
    mu = jnp.mean(xf, axis=-1, keepdims=True)
    var = jnp.mean(jnp.square(xf - mu), axis=-1, keepdims=True)
    return ((xf - mu) * lax.rsqrt(var + eps)).astype(x.dtype)


def neighbourhood_attention(q, k, v, rpb):
    bn, s, _ = q.shape
    rows = s // GRID_W
    kh = min(NA_KH, rows)
    shp = (bn, rows, GRID_W, A_HEADS, HEAD_DIM)
    q, k, v = q.reshape(shp), k.reshape(shp), v.reshape(shp)
    ridx = jnp.arange(rows)
    rstart = jnp.clip(ridx - kh // 2, 0, rows - kh)
    key_rows = rstart[:, None] + jnp.arange(kh)[None, :]
    k_rows = k[:, key_rows]
    v_rows = v[:, key_rows]
    scores = jnp.einsum('brqhd,brkchd->brhqkc', q, k_rows).astype(jnp.float32) * (HEAD_DIM ** -0.5)
    col = jnp.arange(GRID_W)
    cstart = jnp.clip(col - NA_KW // 2, 0, GRID_W - NA_KW)
    in_win = (col[None, :] >= cstart[:, None]) & (col[None, :] < cstart[:, None] + NA_KW)
    dr = key_rows - ridx[:, None] + (NA_KH - 1)
    dc = jnp.clip(col[None, :] - col[:, None], -(NA_KW - 1), NA_KW - 1) + (NA_KW - 1)
    bias = rpb.astype(jnp.float32)[:, dr][..., dc]
    scores = scores + jnp.transpose(bias, (1, 0, 3, 2, 4))[None]
    scores = jnp.where(in_win[:, None, :], scores, NEG_INF)
    p = jax.nn.softmax(scores, axis=(-2, -1)).astype(v.dtype)
    out = jnp.einsum('brhqkc,brkchd->brqhd', p, v_rows)
    return out.reshape(bn, s, A_WIDTH)


def centred_conv3(z, w):
    zp = jnp.pad(z, ((0, 0), (1, 1), (0, 0)))
    return zp[:, :-2] * w[0] + zp[:, 1:-1] * w[1] + zp[:, 2:] * w[2]


def _heads(z):
    return z.reshape(z.shape[:-1] + (B_HEADS, HEAD_DIM))


def _rwkv_step(state, inp):
    r, w, k, v, kk, a = inp
    sa = jnp.einsum('dbhij,dbhj->dbhi', state, -kk)
    state = (state * w[..., None, :] + sa[..., :, None] * (kk * a)[..., None, :]
             + v[..., :, None] * k[..., None, :])
    y = jnp.einsum('dbhij,dbhj->dbhi', state, r)
    return state, y


def rwkv7_bidirectional(rkv, wl, al, gl, conv_w, w0, w_up, a0, a_up, g_up, k_k, k_a, r_k, gn_gain, gn_bias):
    bn, s, _ = rkv.shape
    dt = rkv.dtype
    rkv = centred_conv3(rkv, conv_w)
    r, k, v = jnp.split(rkv, 3, axis=-1)
    w = jnp.exp(-DECAY_SCALE * jax.nn.sigmoid(
        (w0[:, None, None, :] + jnp.einsum('bsr,nrc->nbsc', jnp.tanh(wl), w_up)).astype(jnp.float32)))
    a = jax.nn.sigmoid((a0[:, None, None, :] + jnp.einsum('bsr,nrc->nbsc', al, a_up)).astype(jnp.float32))
    g = (jax.nn.sigmoid(gl) @ g_up).astype(jnp.float32)
    rf = _heads(r.astype(jnp.float32))
    kf = _heads(k.astype(jnp.float32))
    vf = _heads(v.astype(jnp.float32))
    kk = kf * _heads(k_k.astype(jnp.float32))
    kk = kk * lax.rsqrt(jnp.maximum(jnp.sum(kk * kk, axis=-1, keepdims=True), 1e-24))
    a_h = _heads(a)
    k_dir = kf[None] * (1.0 + (a_h - 1.0) * _heads(k_a.astype(jnp.float32)))

    def to_scan(z):
        z = jnp.broadcast_to(z, (2,) + z.shape[-4:])
        z = jnp.stack([z[0], z[1][:, ::-1]])
        return jnp.moveaxis(z, 2, 0)

    xs = (to_scan(rf), to_scan(_heads(w)), to_scan(k_dir), to_scan(vf), to_scan(kk), to_scan(a_h))
    state0 = jnp.zeros((2, bn, B_HEADS, HEAD_DIM, HEAD_DIM), jnp.float32)
    _, ys = lax.scan(_rwkv_step, state0, xs)
    ys = jnp.moveaxis(ys, 0, 2)
    y = ys[0] + ys[1][:, ::-1]
    mu = jnp.mean(y, axis=-1, keepdims=True)
    var = jnp.mean(jnp.square(y - mu), axis=-1, keepdims=True)
    yn = ((y - mu) * lax.rsqrt(var + GN_EPS)).reshape(bn, s, B_WIDTH) * gn_gain + gn_bias
    bonus = jnp.sum(jnp.sum(rf[None] * k_dir * r_k.astype(jnp.float32), axis=-1, keepdims=True) * vf[None], axis=0)
    return ((yn + bonus.reshape(bn, s, B_WIDTH)) * g).astype(dt)


def multiscale_pool(p, pool_w, pool_scale):
    bn, s, cdim = p.shape
    pf = p.astype(jnp.float32)
    cs = jnp.concatenate([jnp.zeros((bn, 1, cdim), jnp.float32), jnp.cumsum(pf, axis=1)], axis=1)
    t = jnp.arange(s)
    outs = []
    for gi, win in enumerate(POOL_WINDOWS):
        lo = jnp.clip(t - win // 2, 0, s - 1)
        hi = jnp.clip(t + win // 2 - 1, 0, s - 1)
        sl = slice(gi * POOL_CH, (gi + 1) * POOL_CH)
        tot = cs[:, hi + 1, sl] - cs[:, lo, sl]
        cnt = (hi - lo + 1).astype(jnp.float32)
        outs.append(tot / cnt[None, :, None] - pf[:, :, sl])
    pooled = jnp.stack(outs, axis=2).astype(p.dtype)
    y = jnp.einsum('bsgc,gce->bsge', pooled, pool_w).reshape(bn, s, cdim)
    return y * pool_scale


def hierarchical_moe(u, w_group, b_group, w_expert, b_expert, w_gate, w_up, w_down):
    bn, s, d = u.shape
    t = bn * s
    xt = u.reshape(t, d)
    gl = (xt @ w_group + b_group).astype(jnp.float32)
    pg = jax.nn.softmax(gl, axis=-1)
    g_idx = jnp.argmax(gl, axis=-1)
    pg_sel = jnp.take_along_axis(pg, g_idx[:, None], axis=-1)[:, 0]
    el = (xt @ w_expert + b_expert).astype(jnp.float32).reshape(t, N_GROUPS, EXPERTS_PER_GROUP)
    el = jnp.take_along_axis(el, g_idx[:, None, None], axis=1)[:, 0]
    top_l, top_i = lax.top_k(el, TOP_K)
    gate = pg_sel[:, None] * jax.nn.softmax(top_l, axis=-1)
    e_flat = (g_idx[:, None] * EXPERTS_PER_GROUP + top_i).reshape(-1)
    w_flat = gate.reshape(-1)
    tok = jnp.repeat(jnp.arange(t), TOP_K)
    n_assign = t * TOP_K
    order = jnp.argsort(e_flat)
    se, stok, sw = e_flat[order], tok[order], w_flat[order]
    counts = jnp.bincount(e_flat, length=N_EXPERTS)
    starts = jnp.cumsum(counts) - counts
    padded = ((counts + MOE_BLOCK - 1) // MOE_BLOCK) * MOE_BLOCK
    pends = jnp.cumsum(padded)
    pstarts = pends - padded
    dest = pstarts[se] + (jnp.arange(n_assign) - starts[se])
    n_blocks = -(-n_assign // MOE_BLOCK) + N_EXPERTS
    total = n_blocks * MOE_BLOCK
    buf_tok = jnp.full((total,), t, jnp.int32).at[dest].set(stok.astype(jnp.int32))
    buf_w = jnp.zeros((total,), jnp.float32).at[dest].set(sw)
    block_e = jnp.clip(jnp.searchsorted(pends, jnp.arange(n_blocks) * MOE_BLOCK, side='right'), 0, N_EXPERTS - 1)
    x_pad = jnp.concatenate([xt, jnp.zeros((1, d), xt.dtype)], axis=0)

    def expert_block(args):
        ti, e, wt = args
        xb = x_pad[ti]
        hb = jax.nn.silu(xb @ w_gate[e]) * (xb @ w_up[e])
        return (hb @ w_down[e]) * wt[:, None]

    y = lax.map(expert_block, (buf_tok.reshape(n_blocks, MOE_BLOCK), block_e,
                               buf_w.reshape(n_blocks, MOE_BLOCK).astype(u.dtype)))
    out = jax.ops.segment_sum(y.reshape(total, d), buf_tok, num_segments=t + 1)[:t]
    return out.reshape(bn, s, d)


def setup_inputs(seed: int = 0) -> dict:
    key = jax.random.key(seed)
    ks = jax.random.split(key, 32)
    f32 = jnp.float32
    L = DEPTH

    def nrm(k, shape, scale):
        return jax.random.normal(k, shape, f32) * scale

    col_scale = (jnp.ones((IN_WIDTH,), f32)
                 .at[2 * A_WIDTH:3 * A_WIDTH].set(BETA)
                 .at[3 * A_WIDTH + 2 * B_WIDTH:3 * A_WIDTH + 3 * B_WIDTH].set(BETA))
    return {
        "x": nrm(ks[0], (BATCH, SEQ, D_MODEL), 1.0),
        "c": nrm(ks[1], (BATCH, D_MODEL), 1.0),
        "w_mod": nrm(ks[2], (L, D_MODEL, 6 * D_MODEL), 0.5 * D_MODEL ** -0.5),
        "b_mod": nrm(ks[3], (L, 6 * D_MODEL), 0.02),
        "w_in": nrm(ks[4], (L, D_MODEL, IN_WIDTH), D_MODEL ** -0.5) * col_scale,
        "na_rpb": nrm(ks[5], (L, A_HEADS, 2 * NA_KH - 1, 2 * NA_KW - 1), 0.1),
        "rw_conv": jnp.array([0.25, 0.5, 0.25], f32)[:, None] + nrm(ks[6], (L, 3, 3 * B_WIDTH), 0.05),
        "rw_w0": jax.random.uniform(ks[7], (L, 2, B_WIDTH), f32, -3.0, 1.0),
        "rw_w_up": nrm(ks[8], (L, 2, R_W, B_WIDTH), 0.5 * R_W ** -0.5),
        "rw_a0": nrm(ks[9], (L, 2, B_WIDTH), 0.5),
        "rw_a_up": nrm(ks[10], (L, 2, R_A, B_WIDTH), 0.5 * R_A ** -0.5),
        "rw_g_up": nrm(ks[11], (L, R_G, B_WIDTH), R_G ** -0.5),
        "rw_k_k": 0.85 + nrm(ks[12], (L, B_WIDTH), 0.05),
        "rw_k_a": 1.0 + nrm(ks[13], (L, B_WIDTH), 0.05),
        "rw_r_k": nrm(ks[14], (L, B_HEADS, HEAD_DIM), 0.1),
        "rw_gn_gain": 1.0 + nrm(ks[15], (L, B_WIDTH), 0.05),
        "rw_gn_bias": nrm(ks[16], (L, B_WIDTH), 0.01),
        "pool_w": nrm(ks[17], (L, N_POOL_GROUPS, POOL_CH, POOL_CH), POOL_CH ** -0.5),
        "pool_scale": 1.0 + nrm(ks[18], (L, C_WIDTH), 0.05),
        "w_out": nrm(ks[19], (L, D_MIX, D_MODEL), BETA * D_MIX ** -0.5),
        "ln1_gain": 1.0 + nrm(ks[20], (L, D_MODEL), 0.05),
        "ln1_bias": nrm(ks[21], (L, D_MODEL), 0.01),
        "ln2_gain": 1.0 + nrm(ks[22], (L, D_MODEL), 0.05),
        "ln2_bias": nrm(ks[23], (L, D_MODEL), 0.01),
        "moe_w_group": nrm(ks[24], (L, D_MODEL, N_GROUPS), D_MODEL ** -0.5),
        "moe_b_group": nrm(ks[25], (L, N_GROUPS), 0.01),
        "moe_w_expert": nrm(ks[26], (L, D_MODEL, N_EXPERTS), D_MODEL ** -0.5),
        "moe_b_expert": nrm(ks[27], (L, N_EXPERTS), 0.01),
        "moe_w_gate": nrm(ks[28], (L, N_EXPERTS, D_MODEL, D_EXPERT), D_MODEL ** -0.5),
        "moe_w_up": nrm(ks[29], (L, N_EXPERTS, D_MODEL, D_EXPERT), BETA * D_MODEL ** -0.5),
        "moe_w_down": nrm(ks[30], (L, N_EXPERTS, D_EXPERT, D_MODEL), BETA * D_EXPERT ** -0.5),
    }


def reference(x, c, w_mod, b_mod, w_in, na_rpb, rw_conv, rw_w0, rw_w_up, rw_a0, rw_a_up, rw_g_up,
              rw_k_k, rw_k_a, rw_r_k, rw_gn_gain, rw_gn_bias, pool_w, pool_scale, w_out,
              ln1_gain, ln1_bias, ln2_gain, ln2_bias, moe_w_group, moe_b_group, moe_w_expert,
              moe_b_expert, moe_w_gate, moe_w_up, moe_w_down):
    o_a = 3 * A_WIDTH
    o_b = o_a + 3 * B_WIDTH
    o_w = o_b + R_W
    o_al = o_w + R_A
    o_g = o_al + R_G
    for l in range(DEPTH):
        mod = jax.nn.silu(c) @ w_mod[l] + b_mod[l]
        sh1, sc1, g1, sh2, sc2, g2 = [m[:, None, :] for m in jnp.split(mod, 6, axis=-1)]
        u = layer_norm(x) * (1.0 + sc1) + sh1
        h = u @ w_in[l]
        qa, ka, va = jnp.split(h[..., :o_a], 3, axis=-1)
        y_a = neighbourhood_attention(qa, ka, va, na_rpb[l])
        y_b = rwkv7_bidirectional(h[..., o_a:o_b], h[..., o_b:o_w], h[..., o_w:o_al], h[..., o_al:o_g],
                                  rw_conv[l], rw_w0[l], rw_w_up[l], rw_a0[l], rw_a_up[l], rw_g_up[l],
                                  rw_k_k[l], rw_k_a[l], rw_r_k[l], rw_gn_gain[l], rw_gn_bias[l])
        y_c = multiscale_pool(h[..., o_g:], pool_w[l], pool_scale[l])
        mix = jnp.concatenate([y_a, y_b.astype(y_a.dtype), y_c.astype(y_a.dtype)], axis=-1) @ w_out[l]
        x = layer_norm(ALPHA * x + g1 * mix) * ln1_gain[l] + ln1_bias[l]
        u2 = layer_norm(x) * (1.0 + sc2) + sh2
        f = hierarchical_moe(u2, moe_w_group[l], moe_b_group[l], moe_w_expert[l], moe_b_expert[l],
                             moe_w_gate[l], moe_w_up[l], moe_w_down[l])
        x = layer_norm(ALPHA * x + g2 * f) * ln2_gain[l] + ln2_bias[l]
    return x
```

```python
import math
import os
import numpy as np
DBG = int(os.environ.get('K_DEBUG', '99'))
import concourse.bass as bass
import concourse.mybir as mybir
from concourse.bass_utils import run_bass_kernel_spmd
from concourse.ap import AP

F32 = mybir.dt.float32
BF16 = mybir.dt.bfloat16
ALU = mybir.AluOpType
AF = mybir.ActivationFunctionType

D = 1024
SEQ = 8192
NB = 2
DEPTH = 2
HD = 64
A_W = 384
B_W = 384
C_W = 256
O_A = 3 * A_W
O_B = O_A + 3 * B_W
O_W = O_B + 32
O_AL = O_W + 32
O_G = O_AL + 64
IN_W = O_G + C_W
DECAY_SCALE = math.exp(-0.5)
GN_EPS = 64e-5
LN_EPS = 1e-5
ALPHA = (2 * DEPTH) ** 0.25
NEG = -30000.0


class Buf:
    def __init__(self, name=""):
        self.name = name
        self.w = []
        self.r = []


class P:
    def __init__(self):
        self.nc = bass.Bass("TRN2", target_bir_lowering=False)
        nc = self.nc
        self.eng = {"pe": nc.tensor, "dve": nc.vector, "act": nc.scalar,
                    "pool": nc.gpsimd, "sp": nc.sync}
        self.ops = []
        self.nid = 0

    def buf(self, name=""):
        return Buf(name)

    def sb(self, name, shape, dt=F32):
        self.nid += 1
        return self.nc.alloc_sbuf_tensor(f"{name}_{self.nid}", list(shape), dt).ap()

    def ps(self, name, shape, dt=F32):
        self.nid += 1
        return self.nc.alloc_psum_tensor(f"{name}_{self.nid}", list(shape), dt).ap()

    def dram(self, name, shape, dt=F32, kind="Internal"):
        return self.nc.dram_tensor(name, list(shape), dt, kind=kind).ap()

    def op(self, eng, fn, r=(), w=(), dma=False, relax=False):
        i = len(self.ops)
        deps = set()
        for b in r:
            deps.update(b.w)
        for b in w:
            deps.update(b.w)
            deps.update(b.r)
        self.ops.append((eng, fn, deps, dma, relax))
        for b in r:
            b.r.append(i)
        for b in w:
            b.w = [i]
            b.r = []
        return i

    def dma(self, q, out, in_, r=(), w=()):
        E = self.eng[q]
        return self.op(q, lambda: E.dma_start(out=out, in_=in_), r=r, w=w, dma=True)

    def finalize(self):
        nc = self.nc
        n = len(self.ops)
        need = [False] * n
        for j, (e, fn, deps, dma, relax) in enumerate(self.ops):
            for i in deps:
                if self.ops[i][3] or self.ops[i][0] != e or not relax:
                    need[i] = True
        esem = {e: nc.alloc_semaphore(f"es_{e}") for e in self.eng}
        ecnt = {e: 0 for e in self.eng}
        NDS = 40
        dsem = [nc.alloc_semaphore(f"ds_{i}") for i in range(NDS)]
        dcnt = [0] * NDS
        dma_i = 0
        ev = [None] * n
        allsems = list(esem.values()) + dsem
        for sm_ in allsems:
            nc.gpsimd.sem_clear(sm_)
        nc.all_engine_barrier()
        waited = {e: {} for e in self.eng}

        def semobj(k):
            return esem[k[1]] if k[0] == "e" else dsem[k[1]]

        for j, (e, fn, deps, dma, relax) in enumerate(self.ops):
            E = self.eng[e]
            req = {}
            for i in deps:
                if ev[i] is None:
                    continue
                if (not self.ops[i][3]) and self.ops[i][0] == e and (relax or e == "pe"):
                    continue
                k, v = ev[i]
                if req.get(k, 0) < v:
                    req[k] = v
            if dma:
                slot = dma_i % NDS
                dma_i += 1
                if dcnt[slot] > 0:
                    k = ("d", slot)
                    req[k] = max(req.get(k, 0), 16 * dcnt[slot])
            for k, v in req.items():
                if waited[e].get(k, 0) < v:
                    E.wait_ge(semobj(k), v)
                    waited[e][k] = v
            ins = fn()
            if dma:
                dcnt[slot] += 1
                ins.then_inc(dsem[slot], 16)
                ev[j] = (("d", slot), 16 * dcnt[slot])
            elif need[j]:
                ecnt[e] += 1
                ins.then_inc(esem[e], 1)
                ev[j] = (("e", e), ecnt[e])
        for slot in range(NDS):
            if dcnt[slot] > 0 and waited["sp"].get(("d", slot), 0) < 16 * dcnt[slot]:
                nc.sync.wait_ge(dsem[slot], 16 * dcnt[slot])
        nc.all_engine_barrier()
        for sm_ in allsems:
            nc.gpsimd.sem_clear(sm_)
        nc.all_engine_barrier()
        return nc


def bcast_rows(ap2d, nparts):
    t = ap2d.tensor
    return AP(t, ap2d.offset, [[0, nparts]] + [list(x) for x in ap2d.ap[1:]])


def emit_mod_rows(p, c_ap, wmod_ap, bmod_ap, ncols, psum, name, wtile=None):
    nc = p.nc
    cb = p.buf()
    ct = p.sb(name + "c", [128, 8])
    p.dma("sp", ct, c_ap, w=[cb])
    sg = p.sb(name + "sg", [128, 8])
    p.op("act", lambda: nc.scalar.activation(out=sg, in_=ct, func=AF.Sigmoid), r=[cb], w=[cb])
    p.op("dve", lambda: nc.vector.tensor_tensor(out=ct, in0=ct, in1=sg, op=ALU.mult), r=[cb], w=[cb])
    ones = p.sb(name + "ones", [1, 128])
    ob = p.buf()
    p.op("dve", lambda: nc.vector.memset(ones, 1.0), w=[ob])
    row = p.sb(name + "row", [1, ncols])
    rb = p.buf()
    brow = p.sb(name + "brow", [1, ncols])
    bb = p.buf()
    p.dma("sp", brow, bmod_ap.rearrange("(o n) -> o n", o=1), w=[bb])
    out = p.sb(name + "bc", [128, ncols])
    outb = p.buf()
    if wtile is None:
        wt = [p.sb(name + "w0", [128, 8, 512])]
        wb = [p.buf()]
    else:
        wt, wb = [wtile[0]], [wtile[1]]
    pr, prb, pb, pbb = psum[0][0:1, :], psum[1], psum[2], psum[3]
    for ci in range(ncols // 512):
        w_, wb_ = wt[0], wb[0]
        cs = slice(ci * 512, (ci + 1) * 512)
        p.dma("sp" if ci % 2 == 0 else "act", w_,
              wmod_ap[:, cs].rearrange("(kc p) n -> p kc n", p=128), w=[wb_])
        for kc in range(8):
            p.op("pe", lambda kc=kc, w_=w_: nc.tensor.matmul(
                pr, ct[:, kc:kc + 1], w_[:, kc, :], start=(kc == 0), stop=(kc == 7)),
                r=[cb, wb_], w=[prb])
        p.op("dve", lambda cs=cs: nc.vector.tensor_tensor(out=row[:, cs], in0=pr, in1=brow[:, cs], op=ALU.add),
             r=[prb, bb], w=[rb])
        p.op("pe", lambda cs=cs: nc.tensor.matmul(pb, ones, row[:, cs], start=True, stop=True),
             r=[rb, ob], w=[pbb])
        p.op("act", lambda cs=cs: nc.scalar.copy(out=out[:, cs], in_=pb), r=[pbb], w=[outb])
    return out, outb


def emit_ln_tile(p, x_t, xb, out_t, outb, scale_bc, shift_bc, bcb, tmp, tmpb, eps=LN_EPS, plus1=True):
    nc = p.nc
    st = tmp["st"]
    mv = tmp["mv"]
    rs = tmp["rs"]
    xn = tmp["xn"]
    F = x_t.shape[-1]
    sc = tmp["sc"]
    p.op("dve", lambda: nc.vector.reduce_sum(out=sc[:, 0:1], in_=x_t, axis=mybir.AxisListType.X), r=[xb], w=[tmpb])
    p.op("dve", lambda: nc.vector.tensor_scalar(out=sc[:, 1:2], in0=sc[:, 0:1], scalar1=-1.0 / F, scalar2=None,
                                                op0=ALU.mult), r=[tmpb], w=[tmpb])
    p.op("dve", lambda: nc.vector.tensor_scalar(out=tmp["sq"], in0=x_t, scalar1=sc[:, 1:2], scalar2=None,
                                                op0=ALU.add), r=[xb, tmpb], w=[tmpb])
    p.op("dve", lambda: nc.vector.scalar_tensor_tensor(out=xn, in0=tmp["sq"], scalar=1.0, in1=tmp["sq"], op0=ALU.mult,
                                                       op1=ALU.mult, accum_out=sc[:, 2:3]), r=[tmpb], w=[tmpb])
    p.op("dve", lambda: nc.vector.tensor_scalar(out=sc[:, 3:4], in0=sc[:, 2:3], scalar1=1.0 / F, scalar2=eps,
                                                op0=ALU.mult, op1=ALU.add), r=[tmpb], w=[tmpb])
    p.op("act", lambda: nc.scalar.activation(out=sc[:, 4:5], in_=sc[:, 3:4], func=AF.Sqrt), r=[tmpb], w=[tmpb])
    p.op("dve", lambda: nc.vector.reciprocal(out=rs, in_=sc[:, 4:5]), r=[tmpb], w=[tmpb])
    p.op("dve", lambda: nc.vector.tensor_scalar(out=xn, in0=tmp["sq"], scalar1=rs, scalar2=None,
                                                op0=ALU.mult), r=[tmpb], w=[tmpb])
    p.op("dve", lambda: nc.vector.tensor_copy(out=mv, in_=sc[:, 1:3]), r=[tmpb], w=[tmpb])
    if plus1:
        p.op("dve", lambda: nc.vector.scalar_tensor_tensor(out=xn, in0=scale_bc, scalar=1.0, in1=xn,
                                                           op0=ALU.add, op1=ALU.mult), r=[bcb, tmpb], w=[tmpb])
    else:
        p.op("dve", lambda: nc.vector.tensor_tensor(out=xn, in0=xn, in1=scale_bc, op=ALU.mult),
             r=[bcb, tmpb], w=[tmpb])
    p.op("dve", lambda: nc.vector.tensor_tensor(out=out_t, in0=xn, in1=shift_bc, op=ALU.add),
         r=[bcb, tmpb], w=[outb])


def ln_tmp(p, name):
    return {"st": p.sb(name + "st", [128, 2, 6]), "mv": p.sb(name + "mv", [128, 2]),
            "rs": p.sb(name + "rs", [128, 1]), "xn": p.sb(name + "xn", [128, 1024]),
            "sq": p.sb(name + "sq", [128, 1024]), "sc": p.sb(name + "sc", [128, 8])}, p.buf()


def build_scan(T=SEQ, SB=4, NRB=4, stop=9, dbg=False):
    IK = "ExternalOutput" if dbg else "Internal"
    p = P()
    nc = p.nc
    NT = T // 128
    xin = p.dram("x", [T, D], kind="ExternalInput")
    c_in = p.dram("c", [128, 8], kind="ExternalInput")
    wmod = p.dram("wmod", [D, 2048], kind="ExternalInput")
    bmod = p.dram("bmod", [2048], kind="ExternalInput")
    w320 = p.dram("w320", [D, 320], kind="ExternalInput")
    cw = p.dram("cw", [3, 192], kind="ExternalInput")
    w0 = p.dram("w0", [2, 64], kind="ExternalInput")
    a0 = p.dram("a0", [2, 64], kind="ExternalInput")
    lowup = p.dram("lowup", [128, 5, 64], kind="ExternalInput")
    vecs = p.dram("vecs", [5, 64], kind="ExternalInput")
    ident_in = p.dram("ident", [128, 128], kind="ExternalInput")
    jmat_in = p.dram("jmat", [128, 128], kind="ExternalInput")
    yout = p.dram("y", [T, 64], kind="ExternalOutput")
    Hs = p.dram("Hs", [T + 2, 192], kind=IK)
    Q = [p.dram("Q%d" % d, [T, 320], kind=IK) for d in range(2)]
    BONs = p.dram("BONs", [T, 64], kind=IK)
    Gs = p.dram("Gs", [T, 64], kind=IK)
    Hsb, Qb, BONb, Gb = p.buf(), [p.buf(), p.buf()], p.buf(), p.buf()

    ident = p.sb("ident", [128, 128])
    jmat = p.sb("jmat", [128, 128])
    identb = p.sb("identb", [128, 128], BF16)
    cb = p.buf()
    p.dma("sp", ident, ident_in, w=[cb])
    jb = p.buf()
    p.dma("sp", jmat, jmat_in, w=[jb])
    ibb = p.buf()
    p.op("dve", lambda: nc.vector.tensor_copy(out=identb, in_=ident), r=[cb], w=[ibb])
    zt = p.sb("zt", [1, 192])
    zb = p.buf()
    p.op("dve", lambda: nc.vector.memset(zt, 0.0), w=[zb])
    hz0, hz1 = p.buf(), p.buf()
    p.dma("sp", Hs[0:1, :], zt, r=[zb], w=[hz0])
    p.dma("sp", Hs[T + 1:T + 2, :], zt, r=[zb], w=[hz1])

    ph = [p.ps("ph%d" % i, [128, 512]) for i in range(2)]
    phb = [p.buf() for _ in range(2)]
    mod, modb = emit_mod_rows(p, c_in, wmod, bmod, 2048, (ph[0], phb[0], ph[1], phb[1]), "m")
    sh1 = mod[:, 0:1024]
    sc1 = mod[:, 1024:2048]

    wf = p.sb("wf", [128, 8, 320])
    wfb = p.buf()
    p.dma("act", wf, w320.rearrange("(kc p) n -> p kc n", p=128), w=[wfb])
    wbf = p.sb("wbf", [128, 8, 320], BF16)
    wbb = p.buf()
    p.op("pool", lambda: nc.gpsimd.tensor_copy(out=wbf, in_=wf), r=[wfb], w=[wbb])
    lu = p.sb("lu", [128, 5, 64])
    lub = p.buf()
    p.dma("sp", lu, lowup, w=[lub])
    cwb_t = p.sb("cwbc", [128, 3, 192])
    w0a0 = p.sb("w0a0", [128, 4, 64])
    vbc = p.sb("vbc", [128, 5, 64])
    bcb = p.buf()
    p.dma("sp", cwb_t, bcast_rows(cw.rearrange("(o a) n -> o a n", o=1), 128), w=[bcb])
    b2 = p.buf()
    p.dma("sp", w0a0[:, 0:2, :], bcast_rows(w0.rearrange("(o a) n -> o a n", o=1), 128), w=[b2])
    b3 = p.buf()
    p.dma("sp", w0a0[:, 2:4, :], bcast_rows(a0.rearrange("(o a) n -> o a n", o=1), 128), w=[b3])
    b4 = p.buf()
    p.dma("sp", vbc, bcast_rows(vecs.rearrange("(o a) n -> o a n", o=1), 128), w=[b4])
    kk_bc, ka_bc, rk_bc, gng_bc, gnb_bc = [vbc[:, i, :] for i in range(5)]

    if stop == 0:
        return p.finalize()
    xt = [p.sb("xt%d" % i, [128, D]) for i in range(2)]
    xb = [p.buf() for _ in range(2)]
    lt, ltb = ln_tmp(p, "ln")
    u = [p.sb("u%d" % i, [128, D], BF16) for i in range(2)]
    ub = [p.buf() for _ in range(2)]
    pT = [p.ps("pT%d" % i, [128, 8, 128], BF16) for i in range(2)]
    pTb = [p.buf() for _ in range(2)]
    uT = [p.sb("uT%d" % i, [128, 8, 128], BF16) for i in range(2)]
    uTb = [p.buf() for _ in range(2)]
    hsb_t = [p.sb("hsb%d" % i, [128, 320]) for i in range(2)]
    hsbb = [p.buf() for _ in range(2)]
    low = [p.sb("low%d" % i, [128, 128]) for i in range(2)]
    lowb = [p.buf() for _ in range(2)]
    pl = p.ps("pl", [128, 128])
    plb = p.buf()
    lowT = [p.sb("lowT%d" % i, [128, 128]) for i in range(2)]
    lowTb = [p.buf() for _ in range(2)]
    pw = p.ps("pw", [128, 5, 64])
    pwb = p.buf()
    pre = [p.sb("pre%d" % i, [128, 5, 64]) for i in range(2)]
    preb = [p.buf() for _ in range(2)]
    PREs = p.dram("PREs", [T, 320], kind=IK)
    PREb = p.buf()
    hstores = []
    if dbg:
        Udbg = p.dram("Udbg", [T, D], BF16, kind="ExternalOutput")
        Mdbg = p.dram("Mdbg", [128, 2048], kind="ExternalOutput")
        p.dma("sp", Mdbg, mod, r=[modb], w=[p.buf()])
    for ti in range(NT):
        k2 = ti % 2
        t0 = ti * 128
        p.dma("sp" if k2 == 0 else "act", xt[k2], xin[t0:t0 + 128, :], w=[xb[k2]])
        emit_ln_tile(p, xt[k2], xb[k2], u[k2], ub[k2], sc1, sh1, modb, lt, ltb)
        if dbg:
            p.dma("sp", Udbg[t0:t0 + 128, :], u[k2], r=[ub[k2]], w=[p.buf()])
        if DBG < 2:
            continue
        for kc in range(8):
            p.op("pe", lambda kc=kc, k2=k2: nc.tensor.transpose(pT[k2][:, kc, :], u[k2][:, kc * 128:(kc + 1) * 128], identb),
                 r=[ub[k2], ibb], w=[pTb[k2]])
        p.op("act", lambda k2=k2: nc.scalar.copy(out=uT[k2], in_=pT[k2]), r=[pTb[k2]], w=[uTb[k2]])
        if DBG < 3:
            continue
        for kc in range(8):
            p.op("pe", lambda kc=kc, k2=k2: nc.tensor.matmul(ph[k2][:, 0:320], uT[k2][:, kc, :], wbf[:, kc, :],
                                                             start=(kc == 0), stop=(kc == 7)),
                 r=[uTb[k2], wbb], w=[phb[k2]])
        if DBG < 4:
            continue
        p.op("act", lambda k2=k2: nc.scalar.copy(out=hsb_t[k2][:, 0:192], in_=ph[k2][:, 0:192]), r=[phb[k2]], w=[hsbb[k2]])
        hb_i = p.buf()
        p.dma("sp", Hs[1 + t0:1 + t0 + 128, :], hsb_t[k2][:, 0:192], r=[hsbb[k2]], w=[hb_i])
        hstores.append(hb_i)
        if DBG < 5:
            continue
        p.op("act", lambda k2=k2: nc.scalar.activation(out=low[k2][:, 0:32], in_=ph[k2][:, 192:224], func=AF.Tanh),
             r=[phb[k2]], w=[lowb[k2]])
        p.op("act", lambda k2=k2: nc.scalar.copy(out=low[k2][:, 32:64], in_=ph[k2][:, 224:256]), r=[phb[k2]], w=[lowb[k2]])
        p.op("act", lambda k2=k2: nc.scalar.activation(out=low[k2][:, 64:128], in_=ph[k2][:, 256:320], func=AF.Sigmoid),
             r=[phb[k2]], w=[lowb[k2]])
        p.op("pe", lambda k2=k2: nc.tensor.matmul(pl, low[k2], ident, start=True, stop=True),
             r=[lowb[k2], cb], w=[plb])
        p.op("dve", lambda k2=k2: nc.vector.tensor_copy(out=lowT[k2], in_=pl), r=[plb], w=[lowTb[k2]])
        if DBG < 6:
            continue
        for q in range(5):
            lo, hi = [(0, 32), (0, 32), (32, 64), (32, 64), (64, 128)][q]
            p.op("pe", lambda q=q, lo=lo, hi=hi, k2=k2: nc.tensor.matmul(
                pw[:, q, :], lowT[k2], lu[:, q, :], start=True, stop=True),
                r=[lowTb[k2], lub], w=[pwb])
        p.op("dve", lambda k2=k2: nc.vector.tensor_copy(out=pre[k2], in_=pw), r=[pwb], w=[preb[k2]])
        pb_i = p.buf()
        p.dma("act", PREs[t0:t0 + 128, :], pre[k2].rearrange("p a b -> p (a b)"), r=[preb[k2]], w=[pb_i])
        hstores.append(pb_i)

    if stop == 1:
        return p.finalize()
    V2 = p.sb("V2", [128, T])
    V2b = p.buf()
    hm = [p.sb("hm%d" % i, [128, 3, 192]) for i in range(2)]
    hmb = [[p.buf() for _ in range(3)] for _ in range(2)]
    prl = [p.sb("prl%d" % i, [128, 5, 64]) for i in range(2)]
    prlb = [p.buf() for _ in range(2)]
    rkv = p.sb("rkv", [128, 192])
    t1 = p.sb("t1", [128, 192])
    t2 = p.sb("t2", [128, 192])
    wk = p.buf()
    qt = [[p.sb("qt%d_%d" % (d, i), [128, 5, 64]) for i in range(2)] for d in range(2)]
    qtb = [[p.buf() for i in range(2)] for d in range(2)]
    sm = p.sb("sm", [128, 8])
    qrev = [p.sb("qrev%d" % i, [128, 320]) for i in range(2)]
    qrevb = [p.buf() for _ in range(2)]
    wa = p.sb("wa", [128, 4, 64])
    kkt = p.sb("kkt", [128, 64])
    junk = p.sb("junk", [128, 64])
    rrk = p.sb("rrk", [128, 64])
    vv = [p.sb("vv%d" % i, [128, 128]) for i in range(2)]
    vvb = [p.buf() for _ in range(2)]
    bon = [p.sb("bon%d" % i, [128, 64]) for i in range(2)]
    bonb = [p.buf() for _ in range(2)]
    pv = [pl, pw.rearrange("p a b -> p (a b)")[:, 0:128]]
    pvb = [plb, pwb]
    for ti in range(NT):
        k2 = ti % 2
        t0 = ti * 128
        for s in range(3):
            p.dma("sp" if s != 1 else "act", hm[k2][:, s, :], Hs[t0 + s:t0 + s + 128, :],
                  r=hstores + [hz0, hz1], w=[hmb[k2][s]])
        p.dma("act", prl[k2], PREs[t0:t0 + 128, :].rearrange("p (a b) -> p a b", a=5), r=hstores, w=[prlb[k2]])
        h_ = hm[k2]
        p.op("dve", lambda h_=h_: nc.vector.tensor_tensor(out=t1, in0=h_[:, 0, :], in1=cwb_t[:, 0, :], op=ALU.mult), r=hmb[k2] + [bcb], w=[wk])
        p.op("dve", lambda h_=h_: nc.vector.tensor_tensor(out=t2, in0=h_[:, 1, :], in1=cwb_t[:, 1, :], op=ALU.mult), r=hmb[k2] + [bcb], w=[wk])
        p.op("dve", lambda: nc.vector.tensor_tensor(out=t1, in0=t1, in1=t2, op=ALU.add), r=[wk], w=[wk])
        p.op("dve", lambda h_=h_: nc.vector.tensor_tensor(out=t2, in0=h_[:, 2, :], in1=cwb_t[:, 2, :], op=ALU.mult), r=hmb[k2] + [bcb], w=[wk])
        p.op("dve", lambda: nc.vector.tensor_tensor(out=rkv, in0=t1, in1=t2, op=ALU.add), r=[wk], w=[wk])
        r_, k_, v_ = rkv[:, 0:64], rkv[:, 64:128], rkv[:, 128:192]
        pr_ = prl[k2]
        p.op("dve", lambda pr_=pr_: nc.vector.tensor_tensor(out=wa, in0=pr_[:, 0:4, :], in1=w0a0, op=ALU.add),
             r=[prlb[k2], b2, b3], w=[wk])
        p.op("act", lambda: nc.scalar.activation(out=wa, in_=wa, func=AF.Sigmoid), r=[wk], w=[wk])
        p.op("dve", lambda: nc.vector.tensor_tensor(out=kkt, in0=k_, in1=kk_bc, op=ALU.mult), r=[wk, b4], w=[wk])
        p.op("dve", lambda: nc.vector.scalar_tensor_tensor(out=junk, in0=kkt, scalar=1.0, in1=kkt, op0=ALU.mult,
                                                           op1=ALU.mult, accum_out=sm[:, 0:1]), r=[wk], w=[wk])
        p.op("dve", lambda: nc.vector.tensor_scalar(out=sm[:, 1:2], in0=sm[:, 0:1], scalar1=1e-24, scalar2=None,
                                                    op0=ALU.max), r=[wk], w=[wk])
        p.op("act", lambda: nc.scalar.activation(out=sm[:, 1:2], in_=sm[:, 1:2], func=AF.Sqrt), r=[wk], w=[wk])
        p.op("dve", lambda: nc.vector.reciprocal(out=sm[:, 1:2], in_=sm[:, 1:2]), r=[wk], w=[wk])
        p.op("dve", lambda: nc.vector.tensor_tensor(out=rrk, in0=r_, in1=rk_bc, op=ALU.mult), r=[wk, b4], w=[wk])
        for d in range(2):
            q_ = qt[d][k2]
            qb_ = qtb[d][k2]
            p.op("dve", lambda q_=q_: nc.vector.tensor_scalar(out=q_[:, 0, :], in0=kkt, scalar1=sm[:, 1:2], scalar2=-1.0,
                                                              op0=ALU.mult, op1=ALU.mult), r=[wk], w=[qb_])
            p.op("act", lambda q_=q_, d=d: nc.scalar.activation(out=q_[:, 1, :], in_=wa[:, d, :], func=AF.Exp,
                                                                scale=-DECAY_SCALE), r=[wk], w=[qb_])
            p.op("dve", lambda q_=q_, d=d: nc.vector.scalar_tensor_tensor(out=q_[:, 2, :], in0=q_[:, 0, :], scalar=-1.0,
                                                                          in1=wa[:, 2 + d, :], op0=ALU.mult, op1=ALU.mult),
                 r=[wk, qb_], w=[qb_])
            p.op("dve", lambda d=d: nc.vector.scalar_tensor_tensor(out=junk, in0=wa[:, 2 + d, :], scalar=-1.0, in1=ka_bc,
                                                                   op0=ALU.add, op1=ALU.mult), r=[wk, b4], w=[wk])
            p.op("dve", lambda q_=q_: nc.vector.scalar_tensor_tensor(out=q_[:, 3, :], in0=junk, scalar=1.0, in1=k_,
                                                                     op0=ALU.add, op1=ALU.mult), r=[wk], w=[qb_])
            p.op("dve", lambda q_=q_: nc.vector.tensor_copy(out=q_[:, 4, :], in_=r_), r=[wk], w=[qb_])
            p.op("dve", lambda q_=q_, d=d: nc.vector.scalar_tensor_tensor(out=junk, in0=rrk, scalar=1.0, in1=q_[:, 3, :],
                                                                          op0=ALU.mult, op1=ALU.mult,
                                                                          accum_out=sm[:, 2 + d:3 + d]), r=[wk, qb_], w=[wk])
            nb_ = p.buf()
            if d == 0:
                p.dma("sp", Q[0][t0:t0 + 128, :], q_.rearrange("p a b -> p (a b)"), r=[qb_], w=[nb_])
            else:
                p.op("pe", lambda q_=q_: nc.tensor.matmul(ph[0][:, 0:320], jmat, q_.rearrange("p a b -> p (a b)"),
                                                          start=True, stop=True), r=[qb_, jb], w=[phb[0]])
                p.op("act", lambda k2=k2: nc.scalar.copy(out=qrev[k2], in_=ph[0][:, 0:320]), r=[phb[0]], w=[qrevb[k2]])
                p.dma("act", Q[1][T - t0 - 128:T - t0, :], qrev[k2], r=[qrevb[k2]], w=[nb_])
            Qb[d].w.extend(nb_.w)
        p.op("dve", lambda: nc.vector.tensor_tensor(out=sm[:, 4:5], in0=sm[:, 2:3], in1=sm[:, 3:4], op=ALU.add), r=[wk], w=[wk])
        p.op("dve", lambda k2=k2: nc.vector.tensor_scalar(out=bon[k2], in0=v_, scalar1=sm[:, 4:5], scalar2=None,
                                                          op0=ALU.mult), r=[wk], w=[bonb[k2]])
        nb_ = p.buf()
        p.dma("sp", BONs[t0:t0 + 128, :], bon[k2], r=[bonb[k2]], w=[nb_])
        BONb.w.extend(nb_.w)
        nb_ = p.buf()
        p.dma("act", Gs[t0:t0 + 128, :], prl[k2][:, 4, :], r=[prlb[k2]], w=[nb_])
        Gb.w.extend(nb_.w)
        p.op("dve", lambda k2=k2: nc.vector.tensor_copy(out=vv[k2][:, 0:64], in_=v_), r=[wk], w=[vvb[k2]])
        p.op("dve", lambda k2=k2: nc.vector.tensor_copy(out=vv[k2][:, 64:128], in_=v_), r=[wk], w=[vvb[k2]])
        p.op("pe", lambda k2=k2: nc.tensor.matmul(pv[0], vv[k2], ident, start=True, stop=True), r=[vvb[k2], cb], w=[pvb[0]])
        p.op("pe", lambda k2=k2: nc.tensor.matmul(pv[1], vv[k2], jmat, start=True, stop=True), r=[vvb[k2], jb], w=[pvb[1]])
        p.op("act", lambda t0=t0: nc.scalar.copy(out=V2[0:64, t0:t0 + 128], in_=pv[0][0:64, :]), r=[pvb[0]], w=[V2b])
        p.op("act", lambda t0=t0: nc.scalar.copy(out=V2[64:128, T - t0 - 128:T - t0], in_=pv[1][64:128, :]), r=[pvb[1]], w=[V2b])

    if stop == 2:
        return p.finalize()
    S = p.sb("S", [128, 64])
    Sb = p.buf()
    y2 = p.sb("y2", [128, T])
    sa = p.sb("sa", [128, 1])
    sj = p.sb("sj", [128, 64])
    p.op("dve", lambda: nc.vector.memset(S, 0.0), w=[Sb])
    rows = [p.sb("rows%d" % i, [128, SB, 320]) for i in range(NRB)]
    rwb = [[p.buf(), p.buf()] for _ in range(NRB)]
    for bi in range(T // SB):
        n0 = bi * SB
        rb = bi % NRB
        R = rows[rb]
        srcf = AP(Q[0].tensor, n0 * 320, [[0, 64], [1, SB * 320]])
        srcb = AP(Q[1].tensor, n0 * 320, [[0, 64], [1, SB * 320]])
        p.dma("sp", R[0:64].rearrange("p a b -> p (a b)"), srcf, r=[Qb[0]], w=[rwb[rb][0]])
        p.dma("sp", R[64:128].rearrange("p a b -> p (a b)"), srcb, r=[Qb[1]], w=[rwb[rb][1]])
        for s in range(SB):
            n = n0 + s
            rr = rwb[rb]
            p.op("dve", lambda R=R, s=s: nc.vector.scalar_tensor_tensor(
                out=sj, in0=S, scalar=1.0, in1=R[:, s, 0:64], op0=ALU.mult, op1=ALU.mult, accum_out=sa), r=rr + [Sb], w=[Sb], relax=True)
            p.op("dve", lambda R=R, s=s: nc.vector.tensor_tensor(out=S, in0=S, in1=R[:, s, 64:128], op=ALU.mult), r=rr, w=[Sb], relax=True)
            p.op("dve", lambda R=R, s=s: nc.vector.scalar_tensor_tensor(
                out=S, in0=R[:, s, 128:192], scalar=sa, in1=S, op0=ALU.mult, op1=ALU.add), r=rr, w=[Sb], relax=True)
            p.op("dve", lambda R=R, s=s, n=n: nc.vector.scalar_tensor_tensor(
                out=S, in0=R[:, s, 192:256], scalar=V2[:, n:n + 1], in1=S, op0=ALU.mult, op1=ALU.add), r=rr + [V2b], w=[Sb], relax=True)
            p.op("dve", lambda R=R, s=s, n=n: nc.vector.scalar_tensor_tensor(
                out=sj, in0=S, scalar=1.0, in1=R[:, s, 256:320], op0=ALU.mult, op1=ALU.mult,
                accum_out=y2[:, n:n + 1]), r=rr, w=[Sb], relax=True)

    if stop == 3:
        return p.finalize()
    py1, py1b, py2, py2b = ph[0][:, 0:64], phb[0], ph[1][:, 0:64], phb[1]
    yb_sb = p.sb("yb_sb", [128, 64])
    ybb = p.buf()
    bg = [p.sb("bg%d" % i, [128, 2, 64]) for i in range(2)]
    bgb = [[p.buf(), p.buf()] for _ in range(2)]
    yo = [p.sb("yo%d" % i, [128, 64]) for i in range(2)]
    yob = [p.buf() for _ in range(2)]
    gst = p.sb("gst", [128, 6])
    gmv = p.sb("gmv", [128, 2])
    grs = p.sb("grs", [128, 1])
    gsq = p.sb("gsq", [128, 64])
    gw = p.buf()
    for ti in range(NT):
        k2 = ti % 2
        t0 = ti * 128
        p.dma("sp", bg[k2][:, 0, :], BONs[t0:t0 + 128, :], r=[BONb], w=[bgb[k2][0]])
        p.dma("act", bg[k2][:, 1, :], Gs[t0:t0 + 128, :], r=[Gb], w=[bgb[k2][1]])
        p.op("pe", lambda t0=t0: nc.tensor.matmul(py2, y2[:, T - 128 - t0:T - t0], ident[:, 64:128], start=True, stop=True),
             r=[Sb, cb], w=[py2b])
        p.op("act", lambda: nc.scalar.copy(out=yb_sb, in_=py2), r=[py2b], w=[ybb])
        p.op("pe", lambda t0=t0: nc.tensor.matmul(py1, y2[:, t0:t0 + 128], ident[:, 0:64], start=True, stop=False),
             r=[Sb, cb], w=[py1b])
        p.op("pe", lambda: nc.tensor.matmul(py1, jmat, yb_sb, start=False, stop=True), r=[ybb, jb], w=[py1b])
        o_ = yo[k2]
        p.op("dve", lambda: nc.vector.reduce_sum(out=gmv[:, 0:1], in_=py1, axis=mybir.AxisListType.X), r=[py1b], w=[gw])
        p.op("dve", lambda: nc.vector.tensor_scalar(out=gmv[:, 0:1], in0=gmv[:, 0:1], scalar1=-1.0 / 64, scalar2=None,
                                                    op0=ALU.mult), r=[gw], w=[gw])
        p.op("dve", lambda o_=o_: nc.vector.tensor_scalar(out=o_, in0=py1, scalar1=gmv[:, 0:1], scalar2=None,
                                                          op0=ALU.add), r=[py1b, gw], w=[yob[k2]])
        p.op("dve", lambda o_=o_: nc.vector.scalar_tensor_tensor(out=gsq, in0=o_, scalar=1.0, in1=o_, op0=ALU.mult,
                                                                 op1=ALU.mult, accum_out=gmv[:, 1:2]), r=[yob[k2]], w=[gw])
        p.op("dve", lambda: nc.vector.tensor_scalar(out=grs, in0=gmv[:, 1:2], scalar1=1.0 / 64, scalar2=GN_EPS,
                                                    op0=ALU.mult, op1=ALU.add), r=[gw], w=[gw])
        p.op("act", lambda: nc.scalar.activation(out=grs, in_=grs, func=AF.Sqrt), r=[gw], w=[gw])
        p.op("dve", lambda: nc.vector.reciprocal(out=grs, in_=grs), r=[gw], w=[gw])
        p.op("dve", lambda o_=o_: nc.vector.tensor_scalar(out=o_, in0=o_, scalar1=grs, scalar2=None,
                                                          op0=ALU.mult), r=[gw], w=[yob[k2]])
        p.op("dve", lambda o_=o_: nc.vector.tensor_tensor(out=o_, in0=o_, in1=gng_bc, op=ALU.mult), r=[b4], w=[yob[k2]])
        p.op("dve", lambda o_=o_: nc.vector.tensor_tensor(out=o_, in0=o_, in1=gnb_bc, op=ALU.add), r=[b4], w=[yob[k2]])
        p.op("dve", lambda o_=o_, k2=k2: nc.vector.tensor_tensor(out=o_, in0=o_, in1=bg[k2][:, 0, :], op=ALU.add),
             r=[bgb[k2][0]], w=[yob[k2]])
        p.op("dve", lambda o_=o_, k2=k2: nc.vector.tensor_tensor(out=o_, in0=o_, in1=bg[k2][:, 1, :], op=ALU.mult),
             r=[bgb[k2][1]], w=[yob[k2]])
        p.dma("sp", yout[t0:t0 + 128, :], o_, r=[yob[k2]], w=[p.buf()])
    return p.finalize()


NW = 2560
NOWN = 2048
HALO = 256
NKC = 6


def na_tile_start(i):
    return min(2 * i, 28)


def na_slot(i):
    return {0: 0, 1: 1, 14: 3, 15: 4}.get(i, 2)


def build_mixer():
    p = P()
    nc = p.nc
    xw = p.dram("xw", [NW, D], kind="ExternalInput")
    c_in = p.dram("c", [128, 8], kind="ExternalInput")
    wmod = p.dram("wmod", [D, 3072], kind="ExternalInput")
    bmod = p.dram("bmod", [3072], kind="ExternalInput")
    wtok = p.dram("wtok", [D, 1408], kind="ExternalInput")
    nab = p.dram("nab", [5, 6, 128, NKC, 128], kind="ExternalInput")
    ybin = p.dram("yb", [NOWN, 384], kind="ExternalInput")
    poolw = p.dram("poolw", [2, 128, 128], kind="ExternalInput")
    pscale = p.dram("pscale", [128, 2], kind="ExternalInput")
    pvalid = p.dram("pvalid", [1, NW], kind="ExternalInput")
    pinv = p.dram("pinv", [2, 128, NOWN], kind="ExternalInput")
    wout = p.dram("wout", [D, D], kind="ExternalInput")
    ln1g = p.dram("ln1g", [1, D], kind="ExternalInput")
    ln1b = p.dram("ln1b", [1, D], kind="ExternalInput")
    ident_in = p.dram("ident", [128, 128], kind="ExternalInput")
    x1out = p.dram("x1", [NOWN, D], kind="ExternalOutput")

    BT = p.ps("BT", [128, 8, 128], BF16)
    BTb = p.buf()
    ACC = [p.ps("ACC%d" % i, [128, 512]) for i in range(3)]
    ACCb = [p.buf() for _ in range(3)]
    SC = p.ps("SC", [128, 8, 128])
    SCb = p.buf()
    PV = p.ps("PV", [128, 512])
    PVb = p.buf()

    ident = p.sb("ident", [128, 128])
    identb = p.sb("identb", [128, 128], BF16)
    cb = p.buf()
    p.dma("sp", ident, ident_in, w=[cb])
    ibb = p.buf()
    p.op("dve", lambda: nc.vector.tensor_copy(out=identb, in_=ident), r=[cb], w=[ibb])

    stga = p.sb("stga", [128, 4096])
    stgab = p.buf()
    mod, modb = emit_mod_rows(p, c_in, wmod, bmod, 3072, (ACC[0], ACCb[0], ACC[1], ACCb[1]), "m",
                              wtile=(stga[:, 0:4096].rearrange("p (a b) -> p a b", a=8), stgab))
    sh1, sc1, g1 = mod[:, 0:1024], mod[:, 1024:2048], mod[:, 2048:3072]

    wbf = p.sb("wbf", [128, 8, 1408], BF16)
    wbb = p.buf()
    stg = [stga[:, 0:2816].rearrange("p (a b) -> p a b", a=2)] * 2
    stgb = [stgab, stgab]
    wtv = wtok.rearrange("(kc p) n -> p kc n", p=128)
    for i in range(4):
        p.dma("sp" if i % 2 == 0 else "act", stg[i % 2], wtv[:, 2 * i:2 * i + 2, :], w=[stgb[i % 2]])
        p.op("pool", lambda i=i: nc.gpsimd.tensor_copy(out=wbf[:, 2 * i:2 * i + 2, :], in_=stg[i % 2]),
             r=[stgb[i % 2]], w=[wbb])
    pwf = p.sb("pwf", [128, 2, 128])
    pwb_ = p.buf()
    p.dma("sp", pwf, poolw.rearrange("c p n -> p c n"), w=[pwb_])
    pwbf = p.sb("pwbf", [128, 2, 128], BF16)
    pwbb = p.buf()
    p.op("dve", lambda: nc.vector.tensor_copy(out=pwbf, in_=pwf), r=[pwb_], w=[pwbb])
    psc = p.sb("psc", [128, 2])
    pscb = p.buf()
    p.dma("sp", psc, pscale, w=[pscb])
    qT = p.sb("qT", [128, 3, NOWN], BF16)
    kT = p.sb("kT", [128, 3, NW], BF16)
    vt = p.sb("vt", [128, NW // 128, 6, 65], BF16)
    pT = p.sb("pT", [128, 2, NW + 16], BF16)
    qb_, kb_, vb_, ptb_ = p.buf(), p.buf(), p.buf(), p.buf()
    qm = [p.sb("qm%d" % i, [128, 128], BF16) for i in range(2)]
    qmb = [p.buf() for _ in range(2)]
    p.op("pool", lambda: nc.gpsimd.memset(qm[0], 0.0), w=[qmb[0]])
    p.op("pool", lambda: nc.gpsimd.memset(qm[1], 0.0), w=[qmb[1]])
    p.op("pool", lambda: nc.gpsimd.memset(vt, 1.0), w=[vb_])
    p.op("pool", lambda: nc.gpsimd.memset(pT, 0.0), w=[ptb_])

    xt = [p.sb("xt%d" % i, [128, D]) for i in range(2)]
    xb = [p.buf() for _ in range(2)]
    lt, ltb = ln_tmp(p, "ln")
    u = [p.sb("u%d" % i, [128, D], BF16) for i in range(2)]
    ub = [p.buf() for _ in range(2)]
    uT1 = p.sb("uT", [128, 8, 512], BF16)
    uTb1 = p.buf()
    val = p.sb("val", [128, 512])
    valb = p.buf()
    acci = 0
    for g in range(NW // 512):
        U_ = uT1
        Ub_ = uTb1
        for tt in range(4):
            ti = g * 4 + tt
            k2 = ti % 2
            p.dma("sp" if k2 == 0 else "act", xt[k2], xw[ti * 128:(ti + 1) * 128, :], w=[xb[k2]])
            emit_ln_tile(p, xt[k2], xb[k2], u[k2], ub[k2], sc1, sh1, modb, lt, ltb)
            for kc in range(8):
                p.op("pe", lambda kc=kc, k2=k2: nc.tensor.transpose(BT[:, kc, :], u[k2][:, kc * 128:(kc + 1) * 128], identb),
                     r=[ub[k2], ibb], w=[BTb])
            p.op("act", lambda U_=U_, tt=tt: nc.scalar.copy(out=U_[:, :, tt * 128:(tt + 1) * 128], in_=BT), r=[BTb], w=[Ub_])
        gs = slice(g * 512, (g + 1) * 512)
        olo, ohi = max(g * 512, HALO), min((g + 1) * 512, HALO + NOWN)
        for j in range(3):
            A_, Ab_ = ACC[acci % 3], ACCb[acci % 3]
            acci += 1
            for kc in range(8):
                p.op("pe", lambda kc=kc, j=j, A_=A_, U_=U_: nc.tensor.matmul(
                    A_, wbf[:, kc, j * 128:(j + 1) * 128], U_[:, kc, :], start=(kc == 0), stop=(kc == 7)),
                    r=[Ub_, wbb], w=[Ab_])
            a0, a1 = olo - g * 512, ohi - g * 512
            o0, o1 = olo - HALO, ohi - HALO
            p.op("act", lambda A_=A_, j=j, a0=a0, a1=a1, o0=o0, o1=o1: nc.scalar.mul(
                out=qT[:, j, o0:o1], in_=A_[:, a0:a1], mul=0.125), r=[Ab_], w=[qb_])
            A_, Ab_ = ACC[acci % 3], ACCb[acci % 3]
            acci += 1
            for kc in range(8):
                p.op("pe", lambda kc=kc, j=j, A_=A_, U_=U_: nc.tensor.matmul(
                    A_, wbf[:, kc, 384 + j * 128:384 + (j + 1) * 128], U_[:, kc, :], start=(kc == 0), stop=(kc == 7)),
                    r=[Ub_, wbb], w=[Ab_])
            p.op("dve", lambda A_=A_, j=j, gs=gs: nc.vector.tensor_copy(out=kT[:, j, gs], in_=A_), r=[Ab_], w=[kb_])
        for tt in range(4):
            A_, Ab_ = ACC[acci % 3], ACCb[acci % 3]
            acci += 1
            for kc in range(8):
                p.op("pe", lambda kc=kc, tt=tt, A_=A_, U_=U_: nc.tensor.matmul(
                    A_[:, 0:384], U_[:, kc, tt * 128:(tt + 1) * 128], wbf[:, kc, 768:1152], start=(kc == 0), stop=(kc == 7)),
                    r=[Ub_, wbb], w=[Ab_])
            p.op("act", lambda A_=A_, g=g, tt=tt: nc.scalar.copy(
                out=vt[:, g * 4 + tt, :, 0:64], in_=A_[:, 0:384].rearrange("p (h d) -> p h d", h=6)), r=[Ab_], w=[vb_])
        p.dma("sp", val, bcast_rows(pvalid[:, gs], 128), w=[valb])
        for ch in range(2):
            A_, Ab_ = ACC[acci % 3], ACCb[acci % 3]
            acci += 1
            for kc in range(8):
                p.op("pe", lambda kc=kc, ch=ch, A_=A_, U_=U_: nc.tensor.matmul(
                    A_, wbf[:, kc, 1152 + ch * 128:1152 + (ch + 1) * 128], U_[:, kc, :], start=(kc == 0), stop=(kc == 7)),
                    r=[Ub_, wbb], w=[Ab_])
            p.op("dve", lambda A_=A_, ch=ch, g=g: nc.vector.tensor_tensor(
                out=pT[:, ch, 8 + g * 512:8 + (g + 1) * 512], in0=A_, in1=val, op=ALU.mult), r=[Ab_, valb], w=[ptb_])

    woutbf = wbf.rearrange("p a b -> p (a b)")[:, 0:8 * D].rearrange("p (a b) -> p a b", a=8)
    wob = wbb
    wov = wout.rearrange("(kc p) n -> p kc n", p=128)
    stgo = [stga[:, i * 2048:(i + 1) * 2048].rearrange("p (a b) -> p a b", a=2) for i in range(2)]
    for i in range(4):
        p.dma("sp" if i % 2 == 0 else "act", stgo[i % 2], wov[:, 2 * i:2 * i + 2, :], w=[stgab])
        p.op("pool", lambda i=i: nc.gpsimd.tensor_copy(out=woutbf[:, 2 * i:2 * i + 2, :], in_=stgo[i % 2]),
             r=[stgab], w=[wbb])
    lnb, lng = mod[:, 0:1024], mod[:, 1024:2048]
    p.dma("sp", lng, bcast_rows(ln1g, 128), w=[modb])
    p.dma("act", lnb, bcast_rows(ln1b, 128), w=[modb])

    GP = 512
    La = p.sb("La", [128, 2, GP + 16])
    Lb2 = p.sb("Lb2", [128, 2, GP + 16])
    Lw = p.buf()
    pinvt = p.sb("pinvt", [128, 2, GP])
    pib = p.buf()
    pooled = p.sb("pooled", [128, 2, GP], BF16)
    pob = p.buf()
    ycT = p.sb("ycT", [128, 2, NOWN], BF16)
    ycb = p.buf()
    TT = ALU.add
    for g in range(NOWN // GP):
        o = 8 + HALO + g * GP
        p.dma("sp", pinvt, pinv[:, :, g * GP:(g + 1) * GP].rearrange("c p n -> p c n"), w=[pib])
        def col(t0, t1):
            return slice(t0 + 8, t1 + 8)
        p.op("dve", lambda o=o: nc.vector.tensor_tensor(out=La[:, :, col(-7, GP + 7)], in0=pT[:, :, o - 8:o + GP + 6],
                                                        in1=pT[:, :, o - 7:o + GP + 7], op=TT), r=[ptb_], w=[Lw])
        p.op("dve", lambda: nc.vector.tensor_tensor(out=Lb2[:, :, col(-6, GP + 6)], in0=La[:, :, col(-7, GP + 5)],
                                                    in1=La[:, :, col(-5, GP + 7)], op=TT), r=[Lw], w=[Lw])
        def fin(src, ch, hf, o=o):
            ps_ = slice(hf * 64, hf * 64 + 64)
            p.op("dve", lambda: nc.vector.tensor_tensor(out=src[ps_, ch, col(0, GP)], in0=src[ps_, ch, col(0, GP)],
                                                        in1=pinvt[ps_, ch, :], op=ALU.mult), r=[Lw, pib], w=[Lw])
            p.op("dve", lambda: nc.vector.tensor_tensor(out=pooled[ps_, ch, :], in0=src[ps_, ch, col(0, GP)],
                                                        in1=pT[ps_, ch, o:o + GP], op=ALU.subtract), r=[Lw, ptb_], w=[pob])
        fin(La, 0, 0)
        fin(Lb2, 0, 1)
        p.op("dve", lambda: nc.vector.tensor_tensor(out=La[:, 1:2, col(-4, GP + 4)], in0=Lb2[:, 1:2, col(-6, GP + 2)],
                                                    in1=Lb2[:, 1:2, col(-2, GP + 6)], op=TT), r=[Lw], w=[Lw])
        p.op("dve", lambda: nc.vector.tensor_tensor(out=Lb2[64:128, 1:2, col(0, GP)], in0=La[64:128, 1:2, col(-4, GP - 4)],
                                                    in1=La[64:128, 1:2, col(4, GP + 4)], op=TT), r=[Lw], w=[Lw])
        fin(La, 1, 0)
        fin(Lb2, 1, 1)
        for ch in range(2):
            A_, Ab_ = ACC[acci % 3], ACCb[acci % 3]
            acci += 1
            p.op("pe", lambda A_=A_, ch=ch: nc.tensor.matmul(A_, pwbf[:, ch, :], pooled[:, ch, :], start=True, stop=True),
                 r=[pob, pwbb], w=[Ab_])
            p.op("act", lambda A_=A_, ch=ch, g=g: nc.scalar.activation(out=ycT[:, ch, g * GP:(g + 1) * GP], in_=A_,
                                                                        func=AF.Copy, scale=psc[:, ch:ch + 1]),
                 r=[Ab_, pscb], w=[ycb])

    bias_t = [p.sb("bias%d" % i, [128, NKC, 128]) for i in range(2)]
    biasb = [p.buf() for _ in range(2)]
    s_t = p.sb("s_t", [128, NKC, 128])
    sb_ = p.buf()
    pt_t = [p.sb("pt%d" % i, [128, NKC, 128], BF16) for i in range(2)]
    ptb2 = [p.buf() for _ in range(2)]
    ya = p.sb("ya", [128, 768], BF16)
    yab = p.buf()
    ybt = p.sb("ybt", [128, 384])
    ybb = p.buf()
    rinv = p.sb("rinv", [128, 1])
    rb_ = p.buf()
    mixT = p.sb("mixT", [128, 6, 128], BF16)
    mixb = p.buf()
    zt = p.sb("zt", [128, D])
    ztb = p.buf()
    x1t = xt[1]
    x1b = xb[1]
    hcount = 0
    for i in range(NOWN // 128):
        s_i = na_tile_start(i)
        slot = na_slot(i)
        qs = slice(i * 128, (i + 1) * 128)
        p.dma("act", ybt, ybin[qs, :], w=[ybb])
        p.dma("sp", xt[0], xw[HALO + i * 128:HALO + (i + 1) * 128, :], w=[xb[0]])
        for h in range(6):
            j = h // 2
            hf = h % 2
            hb = hcount % 2
            hcount += 1
            p.dma("sp" if h % 2 == 0 else "act", bias_t[hb], nab[slot, h], w=[biasb[hb]])
            ps_ = slice(hf * 64, hf * 64 + 64)
            p.op("act", lambda j=j, hf=hf, ps_=ps_, qs=qs: nc.scalar.copy(out=qm[hf][ps_, :], in_=qT[ps_, j, qs]),
                 r=[qb_], w=[qmb[hf]])
            for c in range(NKC):
                k0 = s_i * 64 + c * 128
                p.op("pe", lambda c=c, k0=k0, j=j, hf=hf: nc.tensor.matmul(
                    SC[:, c, :], kT[:, j, k0:k0 + 128], qm[hf], start=True, stop=True),
                    r=[kb_, qmb[hf]], w=[SCb])
            p.op("dve", lambda hb=hb: nc.vector.tensor_tensor(out=s_t, in0=SC[:, 0:NKC, :], in1=bias_t[hb], op=ALU.add),
                 r=[SCb, biasb[hb]], w=[sb_])
            p.op("act", lambda hb=hb: nc.scalar.activation(out=pt_t[hb], in_=s_t, func=AF.Exp), r=[sb_], w=[ptb2[hb]])
            for c in range(NKC):
                wt_ = s_i // 2 + c
                p.op("pe", lambda c=c, wt_=wt_, h=h, hb=hb: nc.tensor.matmul(
                    PV[:, 0:65], pt_t[hb][:, c, :], vt[:, wt_, h, :], start=(c == 0), stop=(c == NKC - 1)),
                    r=[ptb2[hb], vb_], w=[PVb])
            p.op("dve", lambda: nc.vector.reciprocal(out=rinv, in_=PV[:, 64:65]), r=[PVb], w=[rb_])
            p.op("dve", lambda h=h: nc.vector.tensor_scalar(out=ya[:, h * 64:(h + 1) * 64], in0=PV[:, 0:64], scalar1=rinv,
                                                            scalar2=None, op0=ALU.mult), r=[PVb, rb_], w=[yab])
        p.op("act", lambda: nc.scalar.copy(out=ya[:, 384:768], in_=ybt), r=[ybb], w=[yab])
        for kc in range(6):
            p.op("pe", lambda kc=kc: nc.tensor.transpose(BT[:, kc, :], ya[:, kc * 128:(kc + 1) * 128], identb),
                 r=[yab, ibb], w=[BTb])
        p.op("act", lambda: nc.scalar.copy(out=mixT, in_=BT[:, 0:6, :]), r=[BTb], w=[mixb])
        for half in range(2):
            A_, Ab_ = ACC[half], ACCb[half]
            for kc in range(8):
                lhs = mixT[:, kc, :] if kc < 6 else ycT[:, kc - 6, qs]
                p.op("pe", lambda kc=kc, lhs=lhs, A_=A_, half=half: nc.tensor.matmul(
                    A_, lhs, woutbf[:, kc, half * 512:(half + 1) * 512], start=(kc == 0), stop=(kc == 7)),
                    r=[mixb, ycb, wob], w=[Ab_])
            hs = slice(half * 512, (half + 1) * 512)
            p.op("dve", lambda A_=A_, hs=hs: nc.vector.tensor_tensor(out=zt[:, hs], in0=A_, in1=g1[:, hs], op=ALU.mult),
                 r=[Ab_, modb], w=[ztb])
        p.op("dve", lambda: nc.vector.scalar_tensor_tensor(out=zt, in0=xt[0], scalar=ALPHA, in1=zt,
                                                           op0=ALU.mult, op1=ALU.add), r=[xb[0], ztb], w=[ztb])
        emit_ln_tile(p, zt, ztb, x1t, x1b, lng, lnb, modb, lt, ltb, plus1=False)
        p.dma("sp", x1out[qs, :], x1t, r=[x1b], w=[p.buf()])
    return p.finalize()


NEXP = 32
DEXP = 512
BIG = 1.0e30


def build_moe():
    p = P()
    nc = p.nc
    x1in = p.dram("x1", [NOWN, D], kind="ExternalInput")
    c_in = p.dram("c", [128, 8], kind="ExternalInput")
    wmod = p.dram("wmod", [D, 3072], kind="ExternalInput")
    bmod = p.dram("bmod", [3072], kind="ExternalInput")
    wr = p.dram("wr", [D, 36], kind="ExternalInput")
    br = p.dram("br", [1, 36], kind="ExternalInput")
    wg = p.dram("wg", [NEXP, D, DEXP], kind="ExternalInput")
    wu = p.dram("wu", [NEXP, D, DEXP], kind="ExternalInput")
    wd = p.dram("wd", [NEXP, DEXP, D], kind="ExternalInput")
    ln2g = p.dram("ln2g", [1, D], kind="ExternalInput")
    ln2b = p.dram("ln2b", [1, D], kind="ExternalInput")
    ident_in = p.dram("ident", [128, 128], kind="ExternalInput")
    x2out = p.dram("x2", [NOWN, D], kind="ExternalOutput")

    BT = p.ps("BT", [128, 8, 128], BF16)
    BTb = p.buf()
    GU = [[p.ps("G%d" % i, [128, 512]), p.ps("U%d" % i, [128, 512])] for i in range(2)]
    GUb = [[p.buf(), p.buf()] for _ in range(2)]
    OO = p.ps("OO", [128, 1024])
    Ob = [p.buf(), p.buf()]

    ident = p.sb("ident", [128, 128])
    identb = p.sb("identb", [128, 128], BF16)
    cb = p.buf()
    p.dma("sp", ident, ident_in, w=[cb])
    ibb = p.buf()
    p.op("dve", lambda: nc.vector.tensor_copy(out=identb, in_=ident), r=[cb], w=[ibb])

    stga = p.sb("stga", [128, 4096])
    stgab = p.buf()
    mod, modb = emit_mod_rows(p, c_in, wmod, bmod, 3072, (GU[0][0], GUb[0][0], GU[0][1], GUb[0][1]), "m",
                              wtile=(stga.rearrange("p (a b) -> p a b", a=8), stgab))
    sh2, sc2, g2 = mod[:, 0:1024], mod[:, 1024:2048], mod[:, 2048:3072]
    stg = [stga[:, 0:2048], stga[:, 2048:4096]]
    stgb = [stgab, p.buf()]

    wrt = p.sb("wrt", [128, 8, 36])
    wrb = p.buf()
    p.dma("sp", wrt, wr.rearrange("(kc p) n -> p kc n", p=128), w=[wrb])
    brt = p.sb("brt", [128, 36])
    brb = p.buf()
    p.dma("sp", brt, bcast_rows(br, 128), w=[brb])
    lgt = p.sb("lgt", [128, 2, D])
    lgb = p.buf()
    p.dma("sp", lgt[:, 0, :], bcast_rows(ln2g, 128), w=[lgb])
    lgb2 = p.buf()
    p.dma("act", lgt[:, 1, :], bcast_rows(ln2b, 128), w=[lgb2])

    NTP = 8
    u2T = p.sb("u2T", [128, 8, NTP * 128], BF16)
    u2Tb = p.buf()
    Wd = p.sb("Wd", [128, NTP, NEXP])
    Wdb = p.buf()
    acc = p.sb("acc", [128, NTP, D])
    accb = [p.buf() for _ in range(NTP)]
    wgb = [p.sb("wgb%d" % i, [128, 8, DEXP], BF16) for i in range(2)]
    wub = [p.sb("wub%d" % i, [128, 8, DEXP], BF16) for i in range(2)]
    wdb = [p.sb("wdb%d" % i, [128, 4, D], BF16) for i in range(2)]
    wgbb = [p.buf() for _ in range(2)]
    wubb = [p.buf() for _ in range(2)]
    wdbb = [p.buf() for _ in range(2)]
    hT = [p.sb("hT%d" % i, [128, 4, 512], BF16) for i in range(2)]
    hTb = [p.buf() for _ in range(2)]
    sg = [p.sb("sg%d" % i, [128, 512]) for i in range(2)]
    sgb = [p.buf() for _ in range(2)]
    xt = [p.sb("xt%d" % i, [128, D]) for i in range(2)]
    xb = [p.buf() for _ in range(2)]
    lt, ltb = ln_tmp(p, "ln")
    uf = p.sb("uf", [128, D])
    ufb = p.buf()
    ubf = p.sb("ubf", [128, D], BF16)
    ubb = p.buf()
    uTf = p.sb("uTf", [128, 8, 128])
    uTfb = p.buf()
    lg = p.sb("lg", [128, 36])
    rs_ = p.sb("rs_", [128, 16])
    og = p.sb("og", [128, 4])
    pen = p.sb("pen", [128, 4])
    elm = p.sb("elm", [128, 32])
    oh1 = p.sb("oh1", [128, 32])
    oh2 = p.sb("oh2", [128, 32])
    ge = p.sb("ge", [128, 4])
    rb_ = p.buf()
    stgi = 0
    for ps_ in range(2):
        for tl in range(NTP):
            ti = ps_ * NTP + tl
            k2 = ti % 2
            p.dma("sp" if k2 == 0 else "act", xt[k2], x1in[ti * 128:(ti + 1) * 128, :], w=[xb[k2]])
            emit_ln_tile(p, xt[k2], xb[k2], uf, ufb, sc2, sh2, modb, lt, ltb)
            p.op("act", lambda: nc.scalar.copy(out=ubf, in_=uf), r=[ufb], w=[ubb])
            for kc in range(8):
                p.op("pe", lambda kc=kc: nc.tensor.transpose(BT[:, kc, :], ubf[:, kc * 128:(kc + 1) * 128], identb),
                     r=[ubb, ibb], w=[BTb])
            p.op("act", lambda tl=tl: nc.scalar.copy(out=u2T[:, :, tl * 128:(tl + 1) * 128], in_=BT), r=[BTb], w=[u2Tb])
            for kc in range(8):
                p.op("pe", lambda kc=kc: nc.tensor.matmul(OO[:, kc * 128:(kc + 1) * 128], uf[:, kc * 128:(kc + 1) * 128], ident,
                                                          start=True, stop=True), r=[ufb, cb], w=Ob)
            p.op("dve", lambda: nc.vector.tensor_copy(out=uTf.rearrange("p a b -> p (a b)"), in_=OO), r=Ob, w=[uTfb])
            LG = GU[1][0][:, 0:36]
            for kc in range(8):
                p.op("pe", lambda kc=kc: nc.tensor.matmul(LG, uTf[:, kc, :], wrt[:, kc, :], start=(kc == 0), stop=(kc == 7)),
                     r=[uTfb, wrb], w=[GUb[1][0]])
            R = [rb_]
            p.op("dve", lambda: nc.vector.tensor_tensor(out=lg, in0=LG, in1=brt, op=ALU.add), r=[GUb[1][0], brb], w=R)
            gmax, ngmax, gsum, pgs, m1, m2, dd, e2, den, g1_, g2_ = [rs_[:, i:i + 1] for i in range(11)]
            p.op("dve", lambda: nc.vector.reduce_max(out=gmax, in_=lg[:, 0:4], axis=mybir.AxisListType.X), r=R, w=R)
            p.op("dve", lambda: nc.vector.tensor_scalar(out=og, in0=lg[:, 0:4], scalar1=gmax, scalar2=None, op0=ALU.is_equal), r=R, w=R)
            p.op("dve", lambda: nc.vector.tensor_scalar(out=ngmax, in0=gmax, scalar1=-1.0, scalar2=None, op0=ALU.mult), r=R, w=R)
            p.op("act", lambda: nc.scalar.activation(out=ge, in_=lg[:, 0:4], func=AF.Exp, bias=ngmax, scale=1.0, accum_out=gsum), r=R, w=R)
            p.op("dve", lambda: nc.vector.reciprocal(out=pgs, in_=gsum), r=R, w=R)
            p.op("dve", lambda: nc.vector.tensor_scalar(out=pen, in0=og, scalar1=-1.0, scalar2=BIG, op0=ALU.add, op1=ALU.mult), r=R, w=R)
            penb = AP(pen.tensor, pen.offset, [list(pen.ap[0]), [1, 4], [0, 8]])
            p.op("dve", lambda penb=penb: nc.vector.tensor_tensor(out=elm.rearrange("p (a b) -> p a b", a=4),
                                                                  in0=lg[:, 4:36].rearrange("p (a b) -> p a b", a=4),
                                                                  in1=penb, op=ALU.add), r=R, w=R)
            p.op("dve", lambda: nc.vector.reduce_max(out=m1, in_=elm, axis=mybir.AxisListType.X), r=R, w=R)
            p.op("dve", lambda: nc.vector.tensor_scalar(out=oh1, in0=elm, scalar1=m1, scalar2=None, op0=ALU.is_equal), r=R, w=R)
            p.op("dve", lambda: nc.vector.scalar_tensor_tensor(out=elm, in0=oh1, scalar=-BIG, in1=elm, op0=ALU.mult, op1=ALU.add), r=R, w=R)
            p.op("dve", lambda: nc.vector.reduce_max(out=m2, in_=elm, axis=mybir.AxisListType.X), r=R, w=R)
            p.op("dve", lambda: nc.vector.tensor_scalar(out=oh2, in0=elm, scalar1=m2, scalar2=None, op0=ALU.is_equal), r=R, w=R)
            p.op("dve", lambda: nc.vector.tensor_tensor(out=dd, in0=m2, in1=m1, op=ALU.subtract), r=R, w=R)
            p.op("act", lambda: nc.scalar.activation(out=e2, in_=dd, func=AF.Exp), r=R, w=R)
            p.op("dve", lambda: nc.vector.tensor_scalar(out=den, in0=e2, scalar1=1.0, scalar2=None, op0=ALU.add), r=R, w=R)
            p.op("dve", lambda: nc.vector.reciprocal(out=den, in_=den), r=R, w=R)
            p.op("dve", lambda: nc.vector.tensor_tensor(out=g1_, in0=pgs, in1=den, op=ALU.mult), r=R, w=R)
            p.op("dve", lambda: nc.vector.tensor_tensor(out=g2_, in0=g1_, in1=e2, op=ALU.mult), r=R, w=R)
            p.op("dve", lambda: nc.vector.tensor_scalar(out=oh1, in0=oh1, scalar1=g1_, scalar2=None, op0=ALU.mult), r=R, w=R)
            p.op("dve", lambda tl=tl: nc.vector.scalar_tensor_tensor(out=Wd[:, tl, :], in0=oh2, scalar=g2_, in1=oh1,
                                                                     op0=ALU.mult, op1=ALU.add), r=R, w=[Wdb])
        for e in range(NEXP):
            eb = e % 2
            for (src, dst, dstb, nk) in ((wg[e].rearrange("(kc p) n -> p kc n", p=128), wgb[eb], wgbb[eb], 8),
                                         (wu[e].rearrange("(kc p) n -> p kc n", p=128), wub[eb], wubb[eb], 8),
                                         (wd[e].rearrange("(hc p) n -> p hc n", p=128), wdb[eb], wdbb[eb], 4)):
                per = 2048 // (src.shape[2])
                for i in range(nk // per):
                    sidx = stgi % 2
                    stgi += 1
                    sv = stg[sidx].rearrange("p (a b) -> p a b", a=per)
                    p.dma("sp", sv, src[:, i * per:(i + 1) * per, :], w=[stgb[sidx]])
                    p.op("pool", lambda sv=sv, dst=dst, i=i, per=per: nc.gpsimd.tensor_copy(out=dst[:, i * per:(i + 1) * per, :], in_=sv),
                         r=[stgb[sidx]], w=[dstb])
            for gq in range(2):
                hb = gq
                for hc in range(4):
                    pb = hc % 2
                    G_, U_ = GU[pb]
                    Gb_, Ub_ = GUb[pb]
                    for kc in range(8):
                        p.op("pe", lambda kc=kc, hc=hc, gq=gq, G_=G_, eb=eb: nc.tensor.matmul(
                            G_, wgb[eb][:, kc, hc * 128:(hc + 1) * 128], u2T[:, kc, gq * 512:(gq + 1) * 512],
                            start=(kc == 0), stop=(kc == 7)), r=[wgbb[eb], u2Tb], w=[Gb_])
                    for kc in range(8):
                        p.op("pe", lambda kc=kc, hc=hc, gq=gq, U_=U_, eb=eb: nc.tensor.matmul(
                            U_, wub[eb][:, kc, hc * 128:(hc + 1) * 128], u2T[:, kc, gq * 512:(gq + 1) * 512],
                            start=(kc == 0), stop=(kc == 7)), r=[wubb[eb], u2Tb], w=[Ub_])
                    p.op("act", lambda G_=G_, pb=pb: nc.scalar.activation(out=sg[pb], in_=G_, func=AF.Silu), r=[Gb_], w=[sgb[pb]])
                    p.op("dve", lambda U_=U_, pb=pb, hb=hb, hc=hc: nc.vector.tensor_tensor(out=hT[hb][:, hc, :], in0=sg[pb], in1=U_,
                                                                                      op=ALU.mult), r=[sgb[pb], Ub_], w=[hTb[hb]])
                for tt in range(4):
                    tl = gq * 4 + tt
                    for half in range(2):
                        O_ = OO[:, half * 512:(half + 1) * 512]
                        for hc in range(4):
                            p.op("pe", lambda hc=hc, tt=tt, half=half, O_=O_, hb=hb, eb=eb: nc.tensor.matmul(
                                O_, hT[hb][:, hc, tt * 128:(tt + 1) * 128], wdb[eb][:, hc, half * 512:(half + 1) * 512],
                                start=(hc == 0), stop=(hc == 3)), r=[hTb[hb], wdbb[eb]], w=[Ob[half]])
                        a_ = acc[:, tl, half * 512:(half + 1) * 512]
                        if e == 0:
                            p.op("dve", lambda O_=O_, a_=a_, tl=tl, e=e: nc.vector.tensor_scalar(
                                out=a_, in0=O_, scalar1=Wd[:, tl, e:e + 1], scalar2=None, op0=ALU.mult),
                                r=[Ob[half], Wdb], w=[accb[tl]])
                        else:
                            p.op("dve", lambda O_=O_, a_=a_, tl=tl, e=e: nc.vector.scalar_tensor_tensor(
                                out=a_, in0=O_, scalar=Wd[:, tl, e:e + 1], in1=a_, op0=ALU.mult, op1=ALU.add),
                                r=[Ob[half], Wdb], w=[accb[tl]])
        for tl in range(NTP):
            ti = ps_ * NTP + tl
            k2 = ti % 2
            p.dma("sp" if k2 == 0 else "act", xt[k2], x1in[ti * 128:(ti + 1) * 128, :], w=[xb[k2]])
            a_ = acc[:, tl, :]
            p.op("dve", lambda a_=a_: nc.vector.tensor_tensor(out=a_, in0=a_, in1=g2, op=ALU.mult), r=[modb], w=[accb[tl]])
            p.op("dve", lambda a_=a_, k2=k2: nc.vector.scalar_tensor_tensor(out=a_, in0=xt[k2], scalar=ALPHA, in1=a_,
                                                                            op0=ALU.mult, op1=ALU.add), r=[xb[k2]], w=[accb[tl]])
            emit_ln_tile(p, a_, accb[tl], uf, ufb, lgt[:, 0, :], lgt[:, 1, :], lgb, lt, ltb, plus1=False)
            p.dma("sp", x2out[ti * 128:(ti + 1) * 128, :], uf, r=[ufb, lgb2], w=[p.buf()])
    return p.finalize()


_IDENT = np.eye(128, dtype=np.float32)
_JMAT = np.ascontiguousarray(_IDENT[::-1])


def _scan_in_map(inp, l, b, h, x_b):
    hc = slice(h * HD, (h + 1) * HD)
    cols = np.concatenate([np.arange(O_A + j * B_W + h * HD, O_A + j * B_W + (h + 1) * HD) for j in range(3)]
                          + [np.arange(O_B, O_G)])
    ccols = np.concatenate([np.arange(j * B_W + h * HD, j * B_W + (h + 1) * HD) for j in range(3)])
    lowup = np.zeros((128, 5, 64), np.float32)
    lowup[0:32, 0] = inp["rw_w_up"][l, 0][:, hc]
    lowup[0:32, 1] = inp["rw_w_up"][l, 1][:, hc]
    lowup[32:64, 2] = inp["rw_a_up"][l, 0][:, hc]
    lowup[32:64, 3] = inp["rw_a_up"][l, 1][:, hc]
    lowup[64:128, 4] = inp["rw_g_up"][l][:, hc]
    vecs = np.stack([inp["rw_k_k"][l][hc], inp["rw_k_a"][l][hc], inp["rw_r_k"][l][h],
                     inp["rw_gn_gain"][l][hc], inp["rw_gn_bias"][l][hc]]).astype(np.float32)
    return {
        "x": np.ascontiguousarray(x_b), "c": np.ascontiguousarray(inp["c"][b].reshape(8, 128).T),
        "wmod": np.ascontiguousarray(inp["w_mod"][l][:, :2048]), "bmod": np.ascontiguousarray(inp["b_mod"][l][:2048]),
        "w320": np.ascontiguousarray(inp["w_in"][l][:, cols]), "cw": np.ascontiguousarray(inp["rw_conv"][l][:, ccols]),
        "w0": np.ascontiguousarray(inp["rw_w0"][l][:, hc]), "a0": np.ascontiguousarray(inp["rw_a0"][l][:, hc]),
        "lowup": lowup, "vecs": vecs, "ident": _IDENT, "jmat": _JMAT,
    }


def na_bias_tables(rpb, q):
    out = np.full((5, 6, 128, NKC, 128), NEG, np.float32)
    for sl, i in enumerate((0, 1, 2, 14, 15)):
        s_i = na_tile_start(i)
        kk = np.arange(NKC * 128)
        kr = 32 * q - 4 + s_i + kk // 64
        kc = kk % 64
        qq = np.arange(128)
        qr = 32 * q + 2 * i + qq // 64
        qc = qq % 64
        rstart = np.clip(qr - 4, 0, 120)
        cstart = np.clip(qc - 8, 0, 48)
        valid = ((kr[:, None] >= 0) & (kr[:, None] < 128) & (kr[:, None] >= rstart[None, :])
                 & (kr[:, None] < rstart[None, :] + 8) & (kc[:, None] >= cstart[None, :])
                 & (kc[:, None] < cstart[None, :] + 16))
        dr = np.clip(kr[:, None] - qr[None, :] + 7, 0, 14)
        dc = np.clip(kc[:, None] - qc[None, :], -15, 15) + 15
        tab = np.where(valid[None], rpb[:, dr, dc], np.float32(NEG)).astype(np.float32)
        out[sl] = tab.reshape(6, NKC, 128, 128).transpose(0, 2, 1, 3)
    return out


def _pool_consts(q):
    t = 2048 * q + np.arange(NOWN)
    pinv = np.zeros((2, 128, NOWN), np.float32)
    for gi, win in enumerate((2, 4, 8, 16)):
        lo = np.clip(t - win // 2, 0, SEQ - 1)
        hi = np.clip(t + win // 2 - 1, 0, SEQ - 1)
        ch, hf = gi // 2, gi % 2
        pinv[ch, hf * 64:(hf + 1) * 64, :] = (1.0 / (hi - lo + 1).astype(np.float32))[None, :]
    wt = 2048 * q - HALO + np.arange(NW)
    pvalid = ((wt >= 0) & (wt < SEQ)).astype(np.float32)[None, :]
    return pinv, pvalid


def _mixer_in_map(inp, l, b, q, x, y_b):
    lo = 2048 * q - HALO
    xw = np.zeros((NW, D), np.float32)
    a, e = max(lo, 0), min(lo + NW, SEQ)
    xw[a - lo:e - lo] = x[b, a:e]
    pw = inp["pool_w"][l]
    poolw = np.zeros((2, 128, 128), np.float32)
    for gi in range(4):
        ch, hf = gi // 2, gi % 2
        poolw[ch, hf * 64:(hf + 1) * 64, hf * 64:(hf + 1) * 64] = pw[gi]
    pinv, pvalid = _pool_consts(q)
    return {
        "xw": xw, "c": np.ascontiguousarray(inp["c"][b].reshape(8, 128).T),
        "wmod": np.ascontiguousarray(inp["w_mod"][l][:, :3072]), "bmod": np.ascontiguousarray(inp["b_mod"][l][:3072]),
        "wtok": np.ascontiguousarray(np.concatenate([inp["w_in"][l][:, 0:O_A], inp["w_in"][l][:, O_G:IN_W]], axis=1)),
        "nab": na_bias_tables(inp["na_rpb"][l], q),
        "yb": np.ascontiguousarray(y_b[b, 2048 * q:2048 * (q + 1), :]),
        "poolw": poolw, "pscale": np.ascontiguousarray(inp["pool_scale"][l].reshape(2, 128).T),
        "pvalid": pvalid, "pinv": pinv, "wout": np.ascontiguousarray(inp["w_out"][l]),
        "ln1g": np.ascontiguousarray(inp["ln1_gain"][l][None, :]), "ln1b": np.ascontiguousarray(inp["ln1_bias"][l][None, :]),
        "ident": _IDENT,
    }


def _moe_in_map(inp, l, b, q, x1):
    return {
        "x1": np.ascontiguousarray(x1[b, 2048 * q:2048 * (q + 1), :]),
        "c": np.ascontiguousarray(inp["c"][b].reshape(8, 128).T),
        "wmod": np.ascontiguousarray(inp["w_mod"][l][:, 3072:]), "bmod": np.ascontiguousarray(inp["b_mod"][l][3072:]),
        "wr": np.ascontiguousarray(np.concatenate([inp["moe_w_group"][l], inp["moe_w_expert"][l]], axis=1)),
        "br": np.ascontiguousarray(np.concatenate([inp["moe_b_group"][l], inp["moe_b_expert"][l]])[None, :]),
        "wg": inp["moe_w_gate"][l], "wu": inp["moe_w_up"][l], "wd": inp["moe_w_down"][l],
        "ln2g": np.ascontiguousarray(inp["ln2_gain"][l][None, :]), "ln2b": np.ascontiguousarray(inp["ln2_bias"][l][None, :]),
        "ident": _IDENT,
    }


def rwkv_branch(inp, l, x):
    pairs = [(b, h) for b in range(NB) for h in range(6)]
    y_b = np.zeros((NB, SEQ, B_W), np.float32)
    for grp in (pairs[0:8], pairs[8:12] + pairs[8:12]):
        nc = build_scan()
        maps = [_scan_in_map(inp, l, b, h, x[b]) for (b, h) in grp]
        res = run_bass_kernel_spmd(nc, maps, core_ids=list(range(8)))
        for (b, h), r in zip(grp, res.results):
            y_b[b, :, h * HD:(h + 1) * HD] = r["y"]
    return y_b


def kernel(**inputs):
    inp = {k: np.asarray(v) for k, v in inputs.items()}
    x = np.ascontiguousarray(inp["x"], dtype=np.float32)
    cores = [(b, q) for b in range(NB) for q in range(4)]
    for l in range(DEPTH):
        y_b = rwkv_branch(inp, l, x)
        nc = build_mixer()
        res = run_bass_kernel_spmd(nc, [_mixer_in_map(inp, l, b, q, x, y_b) for (b, q) in cores], core_ids=list(range(8)))
        x1 = np.zeros_like(x)
        for (b, q), r in zip(cores, res.results):
            x1[b, 2048 * q:2048 * (q + 1), :] = r["x1"]
        nc = build_moe()
        res = run_bass_kernel_spmd(nc, [_moe_in_map(inp, l, b, q, x1) for (b, q) in cores], core_ids=list(range(8)))
        x = np.zeros_like(x)
        for (b, q), r in zip(cores, res.results):
            x[b, 2048 * q:2048 * (q + 1), :] = r["x2"]
    return x
```
